# Optimizing a Trainium2 kernel written in Bass

```python
import jax, jax.numpy as jnp
from jax import lax
import numpy as np

D_MODEL = 1024
BATCH = 4
SEQ = 8192
DEPTH = 1

HEAD_DIM = 64
FOX_HEADS = 8
SWA_HEADS = 8
SWA_KV_HEADS = 2
SWA_GROUP = SWA_HEADS // SWA_KV_HEADS
WINDOW = 128
Q_BLOCK = 128
ROPE_THETA = 10000.0
N_GROUPS = 8
EXPERTS_PER_GROUP = 8
N_EXPERTS = N_GROUPS * EXPERTS_PER_GROUP
TOP_K = 2
D_EXPERT = D_MODEL // 2
MOE_BLOCK = 128
EPS = 1e-6

FOX_W = FOX_HEADS * HEAD_DIM
SWA_W = SWA_HEADS * HEAD_DIM
SWA_KV_W = SWA_KV_HEADS * HEAD_DIM
IN_SPLITS = (FOX_W, FOX_W, FOX_W, FOX_HEADS, SWA_W, SWA_KV_W, SWA_KV_W, D_MODEL, D_MODEL)
N_IN = sum(IN_SPLITS)
SPLIT_POINTS = tuple(int(v) for v in np.cumsum(IN_SPLITS)[:-1])

kernel_name = "hybrid_fox_swa_sink_hier_moe"


def rms_norm(x, g):
    xf = x.astype(jnp.float32)
    y = xf * lax.rsqrt(jnp.mean(xf * xf, axis=-1, keepdims=True) + EPS)
    return (y * g.astype(jnp.float32)).astype(x.dtype)


def rotary(x, positions):
    half = HEAD_DIM // 2
    inv_freq = ROPE_THETA ** (-jnp.arange(half, dtype=jnp.float32) * 2.0 / HEAD_DIM)
    ang = positions.astype(jnp.float32)[..., None] * inv_freq
    cos = jnp.cos(ang)[:, :, None, :]
    sin = jnp.sin(ang)[:, :, None, :]
    xf = x.astype(jnp.float32)
    x1, x2 = xf[..., :half], xf[..., half:]
    return jnp.concatenate([x1 * cos - x2 * sin, x2 * cos + x1 * sin], axis=-1).astype(x.dtype)


def fox_attention(q, k, v, log_f):
    B, S, H, Dh = q.shape
    nb = S // Q_BLOCK
    c = jnp.cumsum(log_f, axis=1)
    c_k = jnp.transpose(c, (0, 2, 1))
    q_blocks = q.reshape(B, nb, Q_BLOCK, H, Dh).transpose(1, 0, 2, 3, 4)
    c_blocks = c.reshape(B, nb, Q_BLOCK, H).transpose(1, 0, 3, 2)
    key_pos = jnp.arange(S)
    scale = HEAD_DIM ** -0.5

    def one_block(args):
        qb, cb, i = args
        s = jnp.einsum('bqhd,bkhd->bhqk', qb, k).astype(jnp.float32) * scale
        s = s + cb[..., None] - c_k[:, :, None, :]
        q_pos = i * Q_BLOCK + jnp.arange(Q_BLOCK)
        causal = key_pos[None, :] <= q_pos[:, None]
        p = jax.nn.softmax(jnp.where(causal, s, -jnp.inf), axis=-1)
        return jnp.einsum('bhqk,bkhd->bqhd', p.astype(v.dtype), v)

    o = lax.map(one_block, (q_blocks, c_blocks, jnp.arange(nb)))
    return o.transpose(1, 0, 2, 3, 4).reshape(B, S, H * Dh)


def swa_sink_attention(q, k, v, sinks):
    B, S, _, Dh = q.shape
    nb = S // WINDOW
    qb = q.reshape(B, nb, WINDOW, SWA_KV_HEADS, SWA_GROUP, Dh)

    def with_prev(t):
        t = t.reshape(B, nb, WINDOW, SWA_KV_HEADS, Dh)
        prev = jnp.pad(t[:, :-1], ((0, 0), (1, 0), (0, 0), (0, 0), (0, 0)))
        return jnp.concatenate([prev, t], axis=2)

    kw, vw = with_prev(k), with_prev(v)
    s = jnp.einsum('bnqhgd,bnkhd->bnhgqk', qb, kw).astype(jnp.float32) * (HEAD_DIM ** -0.5)
    i = jnp.arange(WINDOW)[:, None]
    j = jnp.arange(2 * WINDOW)[None, :]
    dist = WINDOW + i - j
    band = (dist >= 0) & (dist < WINDOW)
    valid = jnp.where((jnp.arange(nb) == 0)[:, None, None], band & (j >= WINDOW), band)
    s = jnp.where(valid[None, :, None, None], s, -jnp.inf)
    sink = jnp.broadcast_to(
        sinks.astype(jnp.float32).reshape(SWA_KV_HEADS, SWA_GROUP)[None, None, :, :, None, None],
        s.shape[:-1] + (1,))
    p = jax.nn.softmax(jnp.concatenate([s, sink], axis=-1), axis=-1)[..., :-1]
    o = jnp.einsum('bnhgqk,bnkhd->bnqhgd', p.astype(v.dtype), vw)
    return o.reshape(B, S, SWA_HEADS * Dh)


def hybrid_mixer(h, positions, w_in, b_forget, b_gate, sinks, w_proj_fox, w_proj_swa, w_out):
    B, S, _ = h.shape
    proj = h @ w_in
    q_f, k_f, v_f, f_logit, q_s, k_s, v_s, gl_f, gl_s = jnp.split(proj, SPLIT_POINTS, axis=-1)
    heads = lambda t, n: t.reshape(B, S, n, HEAD_DIM)
    log_f = jax.nn.log_sigmoid(f_logit.astype(jnp.float32) + b_forget.astype(jnp.float32))
    o_f = fox_attention(heads(q_f, FOX_HEADS), heads(k_f, FOX_HEADS), heads(v_f, FOX_HEADS), log_f)
    o_s = swa_sink_attention(rotary(heads(q_s, SWA_HEADS), positions),
                             rotary(heads(k_s, SWA_KV_HEADS), positions),
                             heads(v_s, SWA_KV_HEADS), sinks)
    g_f = jax.nn.sigmoid(gl_f + b_gate[0])
    g_s = jax.nn.sigmoid(gl_s + b_gate[1])
    merged = g_f * (o_f @ w_proj_fox) + g_s * (o_s @ w_proj_swa)
    return merged @ w_out


def hier_moe(h, w_group, b_group, w_expert, b_expert, w1, w3, w2):
    B, S, D = h.shape
    T = B * S
    hf = h.reshape(T, D)
    g_prob = jax.nn.softmax((hf @ w_group).astype(jnp.float32) + b_group.astype(jnp.float32), axis=-1)
    g_w, g_idx = lax.top_k(g_prob, 1)
    e_logits = ((hf @ w_expert).astype(jnp.float32) + b_expert.astype(jnp.float32)
                ).reshape(T, N_GROUPS, EXPERTS_PER_GROUP)
    e_sel = e_logits[jnp.arange(T), g_idx[:, 0]]
    top_v, top_i = lax.top_k(e_sel, TOP_K)
    gate = (g_w * jax.nn.softmax(top_v, axis=-1)).reshape(-1)
    expert_id = (g_idx * EXPERTS_PER_GROUP + top_i).reshape(-1)
    token_id = jnp.repeat(jnp.arange(T, dtype=jnp.int32), TOP_K)
    A = T * TOP_K
    order = jnp.argsort(expert_id)
    e_sorted = expert_id[order]
    counts = jnp.bincount(expert_id, length=N_EXPERTS)
    padded = (counts + MOE_BLOCK - 1) // MOE_BLOCK * MOE_BLOCK
    start = jnp.cumsum(counts) - counts
    pad_end = jnp.cumsum(padded)
    pad_start = pad_end - padded
    dest = pad_start[e_sorted] + jnp.arange(A) - start[e_sorted]
    P = A + N_EXPERTS * MOE_BLOCK
    n_blk = P // MOE_BLOCK
    row_token = jnp.full((P,), T, jnp.int32).at[dest].set(token_id[order])
    row_gate = jnp.zeros((P,), jnp.float32).at[dest].set(gate[order])
    blk_expert = jnp.minimum(jnp.searchsorted(pad_end, jnp.arange(n_blk) * MOE_BLOCK, side='right'),
                             N_EXPERTS - 1)
    h_pad = jnp.concatenate([hf, jnp.zeros((1, D), hf.dtype)], axis=0)
    xs = h_pad[row_token].reshape(n_blk, MOE_BLOCK, D)

    def expert_block(args):
        xb, e = args
        return (jax.nn.silu(xb @ w1[e]) * (xb @ w3[e])) @ w2[e]

    ys = lax.map(expert_block, (xs, blk_expert)).reshape(P, D)
    out = jnp.zeros((T + 1, D), jnp.float32).at[row_token].add(row_gate[:, None] * ys.astype(jnp.float32))
    return out[:T].reshape(B, S, D).astype(h.dtype)


def setup_inputs(seed: int = 0) -> dict:
    key = jax.random.key(seed)
    ks = jax.random.split(key, 20)
    L, D = DEPTH, D_MODEL
    nrm = lambda k, shape, fan_in: jax.random.normal(k, shape, jnp.float32) * fan_in ** -0.5
    return {
        "x": jax.random.normal(ks[0], (BATCH, SEQ, D), jnp.float32),
        "positions": (jnp.arange(SEQ, dtype=jnp.int32)[None, :]
                      + jax.random.randint(ks[1], (BATCH, 1), 0, 1024, jnp.int32)),
        "attn_norm": 1.0 + 0.1 * jax.random.normal(ks[2], (L, D), jnp.float32),
        "w_in": nrm(ks[3], (L, D, N_IN), D),
        "b_forget": jax.random.uniform(ks[4], (L, FOX_HEADS), jnp.float32, 1.0, 4.0),
        "b_gate": 0.02 * jax.random.normal(ks[5], (L, 2, D), jnp.float32),
        "attn_sinks": jax.random.normal(ks[6], (L, SWA_HEADS), jnp.float32),
        "w_proj_fox": nrm(ks[7], (L, FOX_W, D), FOX_W),
        "w_proj_swa": nrm(ks[8], (L, SWA_W, D), SWA_W),
        "w_out": nrm(ks[9], (L, D, D), D),
        "ffn_norm": 1.0 + 0.1 * jax.random.normal(ks[10], (L, D), jnp.float32),
        "w_group": nrm(ks[11], (L, D, N_GROUPS), D),
        "b_group": 0.01 * jax.random.normal(ks[12], (L, N_GROUPS), jnp.float32),
        "w_expert": nrm(ks[13], (L, D, N_EXPERTS), D),
        "b_expert": 0.01 * jax.random.normal(ks[14], (L, N_EXPERTS), jnp.float32),
        "w1": nrm(ks[15], (L, N_EXPERTS, D, D_EXPERT), D),
        "w3": nrm(ks[16], (L, N_EXPERTS, D, D_EXPERT), D),
        "w2": nrm(ks[17], (L, N_EXPERTS, D_EXPERT, D), D_EXPERT),
        "final_norm": 1.0 + 0.1 * jax.random.normal(ks[18], (D,), jnp.float32),
    }


def reference(x, positions, attn_norm, w_in, b_forget, b_gate, attn_sinks, w_proj_fox, w_proj_swa,
              w_out, ffn_norm, w_group, b_group, w_expert, b_expert, w1, w3, w2, final_norm):
    for l in range(DEPTH):
        h = rms_norm(x, attn_norm[l])
        x = x + hybrid_mixer(h, positions, w_in[l], b_forget[l], b_gate[l], attn_sinks[l],
                             w_proj_fox[l], w_proj_swa[l], w_out[l])
        h = rms_norm(x, ffn_norm[l])
        x = x + hier_moe(h, w_group[l], b_group[l], w_expert[l], b_expert[l], w1[l], w3[l], w2[l])
    return rms_norm(x, final_norm)
```

```python
import numpy as np
from contextlib import ExitStack
import concourse.bass as bass
import concourse.mybir as mybir
from concourse.bass_utils import run_bass_kernel_spmd

F32 = mybir.dt.float32
BF16 = mybir.dt.bfloat16
I32 = mybir.dt.int32
F32R = mybir.dt.float32r
ALU = mybir.AluOpType
AF = mybir.ActivationFunctionType
AX = mybir.AxisListType

COMPUTE = ("pe", "act", "dve", "pool")
NDMA_SEMS = 20

D = 1024
SV = 8192
NSB = 16
NOWN = 8
TOWN = 4096
NT = 64
NTO = 32
NEXP = 64


def configure(nsb=16, nexp=64):
    global SV, NSB, NOWN, TOWN, NT, NTO, NEXP
    NSB = nsb
    SV = nsb * 512
    NOWN = nsb // 2
    TOWN = NOWN * 512
    NT = nsb * 4
    NTO = NOWN * 4
    NEXP = nexp

NIN = 4360
CAP = 256
NSLOT = 64 * CAP
EPS = 1e-6
NEG = -30000.0
TWO_PI = 6.283185307179586
CW1 = 6.28125
CW2 = TWO_PI - CW1


class _Rec:
    def __init__(self):
        self.call = None

    def __getattr__(self, name):
        def f(*args, **kw):
            assert self.call is None
            self.call = (name, args, kw)
            return None
        return f


def _bind(fn):
    r = _Rec()
    fn(r)
    assert r.call is not None
    return r.call


class Sched:
    def __init__(self, nc, es):
        self.nc = nc
        self.ops = {e: [] for e in ("pe", "act", "dve", "pool", "sp")}
        self.sem = {}
        for e in COMPUTE:
            self.sem[e] = es.enter_context(nc.semaphore("s_" + e))
        self.cnt = {e: 0 for e in COMPUTE}
        self.dsem = {}
        self.dcnt = {}
        self.dnext = {}
        for q in ("sp", "pool"):
            self.dsem[q] = [es.enter_context(nc.semaphore(f"d_{q}{i}")) for i in range(NDMA_SEMS)]
            self.dcnt[q] = [0] * NDMA_SEMS
            self.dnext[q] = 0
        self.lastw = {}
        self.readers = {}
        self.waited = {e: {} for e in self.ops}

    def _semh(self, key):
        if key[0] == "c":
            return self.sem[key[1]]
        return self.dsem[key[1]][key[2]]

    def _deps(self, eng, reads, writes):
        deps = {}

        def add(k, v):
            if deps.get(k, 0) < v:
                deps[k] = v
        for t in reads:
            w = self.lastw.get(t)
            if isinstance(w, dict):
                for k_, v_ in w.items():
                    add(k_, v_)
            elif w:
                add(*w)
        for t in writes:
            w = self.lastw.get(t)
            if isinstance(w, dict):
                for k_, v_ in w.items():
                    add(k_, v_)
            elif w:
                add(*w)
            rd = self.readers.get(t, ())
            if isinstance(rd, dict):
                for k_, v_ in rd.items():
                    add(k_, v_)
            else:
                for r in rd:
                    add(*r)
        return deps

    def _filter(self, eng, deps):
        out = []
        for k, v in deps.items():
            if self.waited[eng].get(k, 0) >= v:
                continue
            self.waited[eng][k] = v
            out.append((k, v))
        return out

    def _commit(self, me, reads, writes):
        for t in writes:
            if t[0] == "+":
                d_ = self.lastw.setdefault(t, {})
                d_[me[0]] = max(d_.get(me[0], 0), me[1])
            else:
                self.lastw[t] = me
                self.readers[t] = []
        for t in reads:
            if t[0] == "+":
                d_ = self.readers.setdefault(t, {})
                d_[me[0]] = max(d_.get(me[0], 0), me[1])
            else:
                self.readers.setdefault(t, []).append(me)

    def op(self, eng, fn, reads=(), writes=()):
        deps = self._deps(eng, reads, writes)
        k = ("c", eng)
        if k in deps:
            v = 0
            if eng != "pe":
                for t in reads:
                    w = self.lastw.get(t)
                    if isinstance(w, dict):
                        v = max(v, w.get(k, 0))
                    elif w and w[0] == k:
                        v = max(v, w[1])
            if v:
                deps[k] = v
            else:
                del deps[k]
        waits = self._filter(eng, deps)
        self.cnt[eng] += 1
        me = (k, self.cnt[eng])
        self.ops[eng].append((waits, _bind(fn), (self.sem[eng], 1)))
        self._commit(me, reads, writes)
        return me

    def dma(self, q, fn, reads=(), writes=()):
        i = self.dnext[q]
        self.dnext[q] = (i + 1) % NDMA_SEMS
        key = ("d", q, i)
        deps = self._deps(q, reads, writes)
        prev = self.dcnt[q][i]
        if prev:
            deps[key] = max(deps.get(key, 0), prev)
        waits = self._filter(q, deps)
        self.dcnt[q][i] += 16
        me = (key, self.dcnt[q][i])
        self.ops[q].append((waits, _bind(fn), (self.dsem[q][i], 16)))
        self._commit(me, reads, writes)
        return me

    def barrier(self):
        for eng in self.ops:
            deps = {}
            for q in self.dsem:
                for i in range(NDMA_SEMS):
                    if self.dcnt[q][i]:
                        deps[("d", q, i)] = self.dcnt[q][i]
            for e in COMPUTE:
                if self.cnt[e] and e != eng:
                    deps[("c", e)] = self.cnt[e]
            waits = self._filter(eng, deps)
            if waits:
                self.ops[eng].append((waits, None, None))

    def finish(self, eng="sp"):
        waits = []
        for q in self.dsem:
            for i in range(NDMA_SEMS):
                if self.dcnt[q][i]:
                    waits.append((("d", q, i), self.dcnt[q][i]))
        for e in COMPUTE:
            if self.cnt[e]:
                waits.append((("c", e), self.cnt[e]))
        self.ops[eng].append((waits, None, None))

    def emit(self):
        nc = self.nc
        with nc.Block() as block:
            regcache = {}

            def run(name):
                def body(e):
                    for waits, fn, inc in self.ops[name]:
                        for k, v in waits:
                            e.wait_ge(self._semh(k), v)
                        if fn is not None:
                            kw = fn[2]
                            if fn[0] == "indirect_dma_start" and isinstance(kw.get("bounds_check"), int):
                                if "bc" not in regcache:
                                    regcache["bc"] = e.to_reg(kw["bounds_check"])
                                kw = dict(kw)
                                kw["bounds_check"] = regcache["bc"]
                            getattr(e, fn[0])(*fn[1], **kw).then_inc(inc[0], inc[1])
                return body
            block.tensor(run("pe"))
            block.scalar(run("act"))
            block.vector(run("dve"))
            block.gpsimd(run("pool"))
            block.sync(run("sp"))


class Arena:
    def __init__(self, nc, es, kib=204):
        self.words = kib * 256
        self.t = es.enter_context(nc.sbuf_tensor("arena", [128, self.words], F32))
        self.off = 0
        self.stack = []
        self.hi = 0

    def mark(self):
        self.stack.append(self.off)

    def release(self):
        self.off = self.stack.pop()

    def alloc(self, shape, dtype):
        shape = [int(s) for s in shape]
        n = int(np.prod(shape))
        esz = 2 if dtype == BF16 else 4
        words = (n * esz + 3) // 4
        words = (words + 15) // 16 * 16
        assert self.off + words <= self.words, f"arena overflow {self.off}+{words}>{self.words}"
        ap = self.t[:, self.off:self.off + words]
        self.off += words
        self.hi = max(self.hi, self.off)
        if dtype != F32:
            ap = ap.bitcast(dtype)
        ap = ap[:, 0:n]
        if len(shape) > 1:
            names = " ".join(f"a{i}" for i in range(len(shape)))
            kw = {f"a{i}": int(s) for i, s in enumerate(shape)}
            ap = ap.rearrange(f"p ({names}) -> p {names}", **kw)
        return ap


def build(stop_after=5, dbg=False):
    nc = bass.Bass("TRN2", target_bir_lowering=False)

    def din(name, shape, dt=F32):
        return nc.dram_tensor(name, list(shape), dt, kind="ExternalInput").ap()

    def dscr(name, shape, dt=F32):
        return nc.dram_tensor(name, list(shape), dt, kind="Internal").ap()

    def dout(name, shape, dt=F32):
        return nc.dram_tensor(name, list(shape), dt, kind="ExternalOutput").ap()

    xv = din("xv", [SV, D])
    posv = din("posv", [1, SV], I32)
    valid = din("valid", [128, NT])
    invf = din("invf", [128, 1])
    w_in = din("w_in", [D, NIN])
    attn_norm = din("attn_norm", [128, 8])
    b_forget = din("b_forget", [8, 1])
    b_gate = din("b_gate", [128, 16])
    sinks = din("sinks", [1, 8])
    w_pf = din("w_pf", [512, D])
    w_ps = din("w_ps", [512, D])
    w_out = din("w_out", [D, D])
    ffn_norm = din("ffn_norm", [1, D])
    wr = din("wr", [D, 72])
    br = din("br", [1, 72])
    if stop_after >= 4:
        w1 = din("w1", [64, D, 512])
        w3 = din("w3", [64, D, 512])
        w2 = din("w2", [64, 512, D])
    final_norm = din("final_norm", [1, D])
    y = dout("y", [TOWN, D])

    KTs = dscr("KTs", [8, 65, SV], BF16)
    QTs = dscr("QTs", [8, 65, TOWN], BF16)
    HTs = dscr("HTs", [NOWN, 128, 8 * 512], BF16)
    OsTs = dscr("OsTs", [NOWN, 128, 4 * 512], BF16)
    OfTs = dscr("OfTs", [8, 64, TOWN], BF16)
    X2s = dscr("X2s", [TOWN, D])
    Xd = dscr("+Xd", [NSLOT, D])
    Yd = dscr("+Yd", [NSLOT, D])

    dbgo = {}
    if dbg:
        dbgo["d_cn"] = dout("d_cn", [128, NT * 8])
        dbgo["d_x2"] = dout("d_x2", [TOWN, D])
        dbgo["d_dst"] = dout("d_dst", [128, NTO * 2], I32)
        dbgo["d_gate"] = dout("d_gate", [128, NTO * 2])

    with ExitStack() as es:
        S = Sched(nc, es)
        A = Arena(nc, es)
        PB = [es.enter_context(nc.psum_tensor(f"pb{i}", [128, 512], F32)).ap() for i in range(8)]

        def pbf(i):
            return PB[i].bitcast(BF16)

        op, dma = S.op, S.dma

        identf = A.alloc([128], F32)
        ident = A.alloc([128], BF16)
        mcur4 = A.alloc([4, 128], BF16)
        mprev4 = A.alloc([4, 128], BF16)
        mtmp = A.alloc([128], F32)
        ones_bf = A.alloc([128], BF16)
        ustrict = A.alloc([128], BF16)
        onesf = A.alloc([512], F32)
        invf_t = A.alloc([1], F32)
        bfneg = A.alloc([1], F32)
        bgt = A.alloc([16], F32)
        esink = A.alloc([8], F32)
        gA = A.alloc([8], F32)
        valid_t = A.alloc([NT], F32)
        Cn = A.alloc([NT, 8], F32)

        op("pool", lambda e: e.memset(identf, 0.0), writes=["identf"])
        op("pool", lambda e: e.affine_select(identf, identf, pattern=[[-1, 128]], compare_op=ALU.not_equal,
                                             fill=1.0, base=0, channel_multiplier=1),
           reads=["identf"], writes=["identf"])
        op("dve", lambda e: e.tensor_copy(ident, identf), reads=["identf"], writes=["ident"])
        op("pool", lambda e: e.memset(mtmp, 0.0), writes=["mtmp"])
        op("pool", lambda e: e.affine_select(mtmp, mtmp, pattern=[[1, 128]], compare_op=ALU.is_ge,
                                             fill=NEG, base=0, channel_multiplier=-1),
           reads=["mtmp"], writes=["mtmp"])
        for r in range(4):
            op("dve", lambda e, r=r: e.tensor_copy(mcur4[:, r, :], mtmp), reads=["mtmp"], writes=["mcur4"])
        op("pool", lambda e: e.memset(mtmp, 0.0), reads=["mcur4"], writes=["mtmp"])
        op("pool", lambda e: e.affine_select(mtmp, mtmp, pattern=[[-1, 128]], compare_op=ALU.is_ge,
                                             fill=NEG, base=-1, channel_multiplier=1),
           reads=["mtmp"], writes=["mtmp"])
        for r in range(4):
            op("dve", lambda e, r=r: e.tensor_copy(mprev4[:, r, :], mtmp), reads=["mtmp"], writes=["mprev4"])
        op("pool", lambda e: e.memset(mtmp, 1.0), reads=["mprev4"], writes=["mtmp"])
        op("pool", lambda e: e.affine_select(mtmp, mtmp, pattern=[[1, 128]], compare_op=ALU.is_ge,
                                             fill=0.0, base=-1, channel_multiplier=-1),
           reads=["mtmp"], writes=["mtmp"])
        op("dve", lambda e: e.tensor_copy(ustrict, mtmp), reads=["mtmp"], writes=["ustrict"])
        op("pool", lambda e: e.memset(ones_bf, 1.0), writes=["ones_bf"])
        op("pool", lambda e: e.memset(onesf, 1.0), writes=["onesf"])
        dma("sp", lambda e: e.dma_start(out=invf_t, in_=invf), writes=["invf"])
        dma("sp", lambda e: e.dma_start(out=bfneg[0:8, :], in_=b_forget), writes=["bfneg"])
        op("dve", lambda e: e.tensor_scalar(bfneg[0:8, :], bfneg[0:8, :], -1.0, None, ALU.mult),
           reads=["bfneg"], writes=["bfneg"])
        dma("sp", lambda e: e.dma_start(out=bgt, in_=b_gate), writes=["bgt"])
        dma("sp", lambda e: e.dma_start(out=esink, in_=sinks.partition_broadcast(128)), writes=["esink"])
        op("act", lambda e: e.activation(esink, esink, AF.Exp), reads=["esink"], writes=["esink"])
        dma("sp", lambda e: e.dma_start(out=gA, in_=attn_norm), writes=["gA"])
        dma("sp", lambda e: e.dma_start(out=valid_t, in_=valid), writes=["valid"])

        A.mark()
        Vr = A.alloc([NT, 8, 65], BF16)
        A.mark()
        NW1 = 2312 + 640
        W1 = A.alloc([8, NW1], BF16)
        op("dve", lambda e: e.tensor_copy(Vr[:, :, :, 64], valid_t.unsqueeze(2).to_broadcast([128, NT, 8])),
           reads=["valid"], writes=["Vr_valid"])

        A.mark()
        st = [A.alloc([2312], F32) for _ in range(2)]
        for c in range(8):
            s_ = st[c % 2]
            tk = f"st{c % 2}"
            dma("sp", lambda e, c=c, s_=s_: e.dma_start(out=s_, in_=w_in[c * 128:(c + 1) * 128, 0:2312]), writes=[tk])
            g = gA[:, c:c + 1]
            en = ["dve", "pool"]
            op("dve", lambda e, c=c, s_=s_, g=g: e.tensor_scalar(W1[:, c, 0:512], s_[:, 0:512], g, 0.125, ALU.mult, ALU.mult),
               reads=[tk, "gA"], writes=["+W1"])
            op("pool", lambda e, c=c, s_=s_, g=g: e.tensor_scalar(W1[:, c, 512:1544], s_[:, 512:1544], g, 1.0, ALU.mult, ALU.mult),
               reads=[tk, "gA"], writes=["+W1"])
            qs_in = s_[:, 1544:2056].rearrange("p (g j t d) -> p j g t d", g=2, j=4, t=2, d=32)
            qs_out = W1[:, c, 1544:2056].rearrange("p (j g t d) -> p j g t d", j=4, g=2, t=2, d=32)
            qr_out = W1[:, c, 2312:2824].rearrange("p (j g t d) -> p j g t d", j=4, g=2, t=2, d=32)
            for t in range(2):
                op("dve", lambda e, t=t, a=qs_out, b=qs_in, g=g: e.tensor_scalar(a[:, :, :, t, :], b[:, :, :, t, :], g, 0.125, ALU.mult, ALU.mult),
                   reads=[tk, "gA"], writes=["+W1"])
            op("pool", lambda e, a=qr_out, b=qs_in, g=g: e.tensor_scalar(a[:, :, :, 0, :], b[:, :, :, 1, :], g, -0.125, ALU.mult, ALU.mult),
               reads=[tk, "gA"], writes=["+W1"])
            op("pool", lambda e, a=qr_out, b=qs_in, g=g: e.tensor_scalar(a[:, :, :, 1, :], b[:, :, :, 0, :], g, 0.125, ALU.mult, ALU.mult),
               reads=[tk, "gA"], writes=["+W1"])
            op("dve", lambda e, c=c, s_=s_, g=g: e.tensor_scalar(W1[:, c, 2056:2312], s_[:, 2056:2312], g, 1.0, ALU.mult, ALU.mult),
               reads=[tk, "gA"], writes=["+W1"])
            ks_in = s_[:, 2056:2184].rearrange("p (k t d) -> p k t d", k=2, t=2, d=32)
            kr_out = W1[:, c, 2824:2952].rearrange("p (k t d) -> p k t d", k=2, t=2, d=32)
            op("pool", lambda e, a=kr_out, b=ks_in, g=g: e.tensor_scalar(a[:, :, 0, :], b[:, :, 1, :], g, -1.0, ALU.mult, ALU.mult),
               reads=[tk, "gA"], writes=["+W1"])
            op("pool", lambda e, a=kr_out, b=ks_in, g=g: e.tensor_scalar(a[:, :, 1, :], b[:, :, 0, :], g, 1.0, ALU.mult, ALU.mult),
               reads=[tk, "gA"], writes=["+W1"])
        A.release()
        S.barrier()

        if dbg:
            dbgo["d_w1"] = dout("d_w1", [128, 8 * NW1], BF16)
            dma("sp", lambda e: e.dma_start(out=dbgo["d_w1"], in_=W1.rearrange("p c n -> p (c n)")), reads=["+W1"])
        onesrow = A.alloc([1024], BF16)
        op("pool", lambda e: e.memset(onesrow[0:8, :], 1.0), writes=["onesrow"])
        for i8 in range(SV // 1024):
            dma("pool", lambda e, i8=i8: e.dma_start(out=KTs[:, 64, i8 * 1024:(i8 + 1) * 1024], in_=onesrow[0:8, :]),
                reads=["onesrow"], writes=["+KTs_ones"])

        xt = [A.alloc([D], F32) for _ in range(3)]
        hn = [A.alloc([D], BF16) for _ in range(2)]
        ss = A.alloc([4], F32)
        rstd = A.alloc([4], F32)
        hT = [A.alloc([8, 512], BF16) for _ in range(2)]
        posi = A.alloc([512], I32)
        ang = A.alloc([512], F32)
        kq = A.alloc([512], F32)
        kqi = A.alloc([512], I32)
        cosT = A.alloc([512], F32)
        sinT = A.alloc([512], F32)
        stg = [A.alloc([512], BF16) for _ in range(4)]
        lsp = A.alloc([512], F32)
        cT = A.alloc([512], F32)
        cTb = A.alloc([512], BF16)
        carry = A.alloc([1], F32)
        KsT = [A.alloc([512], BF16) for _ in range(2)]
        Vs = [A.alloc([4, 2, 65], BF16) for _ in range(2)]
        qsT = A.alloc([4, 512], BF16)
        rt1 = A.alloc([512], F32)
        rt2 = A.alloc([512], F32)
        Pt = A.alloc([2, 512], BF16)
        den = A.alloc([4], F32)
        os_t = A.alloc([512], BF16)
        OsT = A.alloc([4, 512], BF16)

        op("pool", lambda e: e.memset(carry, 0.0), writes=["carry"])

        def load_x(i):
            dma("sp", lambda e, i=i: e.dma_start(out=xt[i % 3], in_=xv[i * 128:(i + 1) * 128, :]),
                writes=[f"xt{i % 3}"])

        PJ = [0, 1, 2, 3]
        PT_, PS_, PO_, PM_ = 4, 5, 6, 7
        pj_i = [0]

        def next_pj():
            b = PJ[pj_i[0] % 4]
            pj_i[0] += 1
            return b

        def fm_group(bank, col0, M, hTs, htk):
            for c in range(8):
                op("pe", lambda e, c=c: e.matmul(PB[bank][0:M, :], lhsT=W1[:, c, col0:col0 + M], rhs=hTs[:, c, :],
                                                 start=(c == 0), stop=(c == 7)),
                   reads=["+W1", htk], writes=[f"B{bank}"])

        load_x(0)
        load_x(1)
        for v in range(NSB):
            own = (v % 2 == 1)
            k = v // 2
            sl = v % 2
            hTs = hT[sl]
            htk = f"hT{sl}"
            for t in range(4):
                i = v * 4 + t
                if i + 2 < NT:
                    load_x(i + 2)
                xts, xtk = xt[i % 3], f"xt{i % 3}"
                hns, hnk = hn[i % 2], f"hn{i % 2}"
                op("act", lambda e, t=t, xts=xts, hns=hns: e.activation(hns, xts, AF.Square, accum_out=ss[:, t:t + 1]),
                   reads=[xtk], writes=[hnk, "ss"])
                op("dve", lambda e, t=t: e.tensor_scalar(rstd[:, t:t + 1], ss[:, t:t + 1], 1.0 / D, EPS, ALU.mult, ALU.add),
                   reads=["ss"], writes=["rstd"])
                op("act", lambda e, t=t: e.activation(rstd[:, t:t + 1], rstd[:, t:t + 1], AF.Ln), reads=["rstd"], writes=["rstd"])
                op("act", lambda e, t=t: e.activation(rstd[:, t:t + 1], rstd[:, t:t + 1], AF.Exp, scale=-0.5), reads=["rstd"], writes=["rstd"])
                op("dve", lambda e, t=t, xts=xts, hns=hns: e.tensor_scalar(hns, xts, rstd[:, t:t + 1], None, ALU.mult),
                   reads=[xtk, "rstd", hnk], writes=[hnk])
                for c in range(8):
                    op("pe", lambda e, c=c, hns=hns: e.transpose(pbf(PT_)[:, c * 128:(c + 1) * 128], hns[:, c * 128:(c + 1) * 128], ident),
                       reads=[hnk, "ident"], writes=[f"B{PT_}"])
                op("act", lambda e, t=t: e.activation(hTs[:, :, t * 128:(t + 1) * 128],
                                                      pbf(PT_).rearrange("p (c n) -> p c n", c=8), AF.Copy),
                   reads=[f"B{PT_}"], writes=[htk])
            if own:
                dma("pool", lambda e, k=k, hTs=hTs: e.dma_start(out=HTs[k], in_=hTs.rearrange("p c n -> p (c n)")),
                    reads=[htk], writes=[f"+HTs{k}"])
            dma("sp", lambda e, v=v: e.dma_start(out=posi, in_=posv[0:1, v * 512:(v + 1) * 512].partition_broadcast(128)),
                writes=["posi"])
            op("dve", lambda e: e.tensor_copy(ang, posi), reads=["posi"], writes=["ang"])
            op("dve", lambda e: e.tensor_scalar(ang, ang, invf_t[:, 0:1], None, ALU.mult), reads=["ang", "invf"], writes=["ang"])
            for (tab, shift) in ((sinT, 0.0), (cosT, np.pi / 2)):
                tk = "sinT" if tab is sinT else "cosT"
                op("dve", lambda e, shift=shift: e.tensor_scalar(kq, ang, shift, 1.0 / TWO_PI, ALU.add, ALU.mult),
                   reads=["ang"], writes=["kq"])
                op("dve", lambda e: e.tensor_copy(kqi, kq), reads=["kq"], writes=["kqi"])
                op("dve", lambda e: e.tensor_copy(rt1, kqi), reads=["kqi"], writes=["rt1"])
                op("dve", lambda e: e.tensor_tensor(rt2, kq, rt1, ALU.subtract), reads=["kq", "rt1"], writes=["rt2"])
                op("dve", lambda e: e.scalar_tensor_tensor(rt1, rt2, 0.5, rt2, ALU.is_gt, ALU.subtract),
                   reads=["rt2"], writes=["rt1"])
                op("dve", lambda e: e.scalar_tensor_tensor(rt2, rt1, 0.5, rt1, ALU.is_gt, ALU.subtract),
                   reads=["rt1"], writes=["rt2"])
                op("act", lambda e, tab=tab: e.activation(tab, rt2, AF.Sin, scale=6.283185005), reads=["rt2"], writes=[tk])

            for n in range(4):
                b = next_pj()
                fm_group(b, 512 + n * 128, 128, hTs, htk)
                sg = stg[n % 4]
                sgk = f"stg{n % 4}"
                op("dve" if n % 2 == 0 else "act",
                   (lambda e, b=b, sg=sg: e.tensor_copy(sg, PB[b])) if n % 2 == 0 else
                   (lambda e, b=b, sg=sg: e.activation(sg, PB[b], AF.Copy)),
                   reads=[f"B{b}"], writes=[sgk])
                for hh in range(2):
                    dma("pool", lambda e, n=n, hh=hh, sg=sg, v=v: e.dma_start(
                        out=KTs[2 * n + hh, 0:64, v * 512:(v + 1) * 512], in_=sg[hh * 64:(hh + 1) * 64, :]),
                        reads=[sgk], writes=[f"+KTs{2 * n + hh}"])
            b = PM_
            fm_group(b, 1536, 8, hTs, htk)
            op("act", lambda e: e.activation(lsp[0:8, :], PB[PM_][0:8, :], AF.Exp, bias=bfneg[0:8, :], scale=-1.0),
               reads=[f"B{PM_}", "bfneg"], writes=["lsp"])
            op("act", lambda e: e.activation(lsp[0:8, :], lsp[0:8, :], AF.Ln, bias=1.0), reads=["lsp"], writes=["lsp"])
            op("dve", lambda e: e.tensor_tensor_scan(cT[0:8, :], onesf[0:8, :], lsp[0:8, :], carry[0:8, :], ALU.mult, ALU.subtract),
               reads=["lsp", "onesf", "carry"], writes=["cT"])
            op("dve", lambda e: e.tensor_copy(carry[0:8, :], cT[0:8, 511:512]), reads=["cT"], writes=["carry"])
            for t in range(4):
                op("pe", lambda e, t=t: e.transpose(PB[PM_][:, 256 + t * 8:256 + (t + 1) * 8], cT[0:8, t * 128:(t + 1) * 128], identf[0:8, 0:8]),
                   reads=["cT", "identf"], writes=[f"B{PM_}"])
            op("dve", lambda e, v=v: e.tensor_copy(Cn[:, v * 4:(v + 1) * 4, :], PB[PM_][:, 256:288].rearrange("p (t h) -> p t h", t=4)),
               reads=[f"B{PM_}"], writes=["Cn"])
            if own:
                op("dve", lambda e: e.tensor_copy(cTb[0:8, :], cT[0:8, :]), reads=["cT"], writes=["cTb"])
                dma("pool", lambda e, k=k: e.dma_start(out=QTs[:, 64, k * 512:(k + 1) * 512], in_=cTb[0:8, :]),
                    reads=["cTb"], writes=["+QTs_c"])
                for n in range(4):
                    b = next_pj()
                    fm_group(b, n * 128, 128, hTs, htk)
                    sg = stg[n % 4]
                    sgk = f"stg{n % 4}"
                    op("dve" if n % 2 == 0 else "act",
                       (lambda e, b=b, sg=sg: e.tensor_copy(sg, PB[b])) if n % 2 == 0 else
                       (lambda e, b=b, sg=sg: e.activation(sg, PB[b], AF.Copy)),
                       reads=[f"B{b}"], writes=[sgk])
                    for hh in range(2):
                        dma("pool", lambda e, n=n, hh=hh, sg=sg, k=k: e.dma_start(
                            out=QTs[2 * n + hh, 0:64, k * 512:(k + 1) * 512], in_=sg[hh * 64:(hh + 1) * 64, :]),
                            reads=[sgk], writes=[f"+QTs{2 * n + hh}"])
            rope_list = [(2056, 2824, KsT[sl], f"KsT{sl}")]
            if own:
                for j in range(4):
                    rope_list.append((1544 + j * 128, 2312 + j * 128, qsT[:, j, :], "qsT"))
            for (c0, c0r, dst, dtk) in rope_list:
                b0 = next_pj()
                b1 = next_pj()
                fm_group(b0, c0, 128, hTs, htk)
                fm_group(b1, c0r, 128, hTs, htk)
                op("dve", lambda e, b0=b0: e.tensor_tensor(rt1, PB[b0], cosT, ALU.mult), reads=[f"B{b0}", "cosT"], writes=["rt1"])
                op("dve", lambda e, b1=b1: e.tensor_tensor(rt2, PB[b1], sinT, ALU.mult), reads=[f"B{b1}", "sinT"], writes=["rt2"])
                op("pool", lambda e, dst=dst: e.tensor_tensor(dst, rt1, rt2, ALU.add), reads=["rt1", "rt2"], writes=[dtk])
            op("dve", lambda e, v=v: e.tensor_copy(Vs[sl][:, :, :, 64], valid_t[:, v * 4:(v + 1) * 4].unsqueeze(2).to_broadcast([128, 4, 2])),
               reads=["valid"], writes=[f"Vs{sl}"])
            for t in range(4):
                b = next_pj()
                for c in range(8):
                    op("pe", lambda e, c=c, t=t, b=b: e.matmul(PB[b], lhsT=hTs[:, c, t * 128:(t + 1) * 128], rhs=W1[:, c, 1024:1536],
                                                               start=(c == 0), stop=(c == 7)),
                       reads=["+W1", htk], writes=[f"B{b}"])
                op("act" if t % 2 else "dve",
                   (lambda e, b=b, t=t, v=v: e.activation(Vr[:, v * 4 + t, :, 0:64], PB[b].rearrange("p (h d) -> p h d", h=8), AF.Copy)) if t % 2 else
                   (lambda e, b=b, t=t, v=v: e.tensor_copy(Vr[:, v * 4 + t, :, 0:64], PB[b].rearrange("p (h d) -> p h d", h=8))),
                   reads=[f"B{b}"], writes=["+Vr"])
                b = next_pj()
                for c in range(8):
                    op("pe", lambda e, c=c, t=t, b=b: e.matmul(PB[b][:, 0:128], lhsT=hTs[:, c, t * 128:(t + 1) * 128], rhs=W1[:, c, 2184:2312],
                                                               start=(c == 0), stop=(c == 7)),
                       reads=["+W1", htk], writes=[f"B{b}"])
                op("dve", lambda e, b=b, t=t: e.tensor_copy(Vs[sl][:, t, :, 0:64], PB[b][:, 0:128].rearrange("p (h d) -> p h d", h=2)),
                   reads=[f"B{b}"], writes=[f"Vs{sl}"])
            if own:
                for qb in range(4):
                    for g in range(2):
                        ps = slice(g * 64, (g + 1) * 64)
                        if qb == 0:
                            kprev = (KsT[1 - sl][ps, 384:512], Vs[1 - sl][:, 3, g, :], f"KsT{1 - sl}", f"Vs{1 - sl}")
                        else:
                            kprev = (KsT[sl][ps, (qb - 1) * 128:qb * 128], Vs[sl][:, qb - 1, g, :], f"KsT{sl}", f"Vs{sl}")
                        kcur = (KsT[sl][ps, qb * 128:(qb + 1) * 128], Vs[sl][:, qb, g, :], f"KsT{sl}", f"Vs{sl}")
                        for i, (kap, vap, ktk, vtk) in enumerate((kprev, kcur)):
                            op("pe", lambda e, kap=kap, qb=qb, ps=ps: e.matmul(
                                PB[PS_], lhsT=kap, rhs=qsT[ps, :, qb * 128:(qb + 1) * 128], start=True, stop=False),
                                reads=[ktk, "qsT"], writes=[f"B{PS_}"])
                            m4 = mprev4 if i == 0 else mcur4
                            op("pe", lambda e, m4=m4: e.matmul(PB[PS_], lhsT=ident, rhs=m4.rearrange("p r n -> p (r n)"),
                                                               start=False, stop=True),
                               reads=["ident", "mcur4", "mprev4"], writes=[f"B{PS_}"])
                            op("act", lambda e, i=i: e.activation(Pt[:, i, :], PB[PS_], AF.Exp),
                               reads=[f"B{PS_}"], writes=[f"Pt{i}"])
                        for j in range(4):
                            for i, (kap, vap, ktk, vtk) in enumerate((kprev, kcur)):
                                op("pe", lambda e, i=i, j=j, vap=vap: e.matmul(
                                    PB[PO_][:, j * 65:(j + 1) * 65], lhsT=Pt[:, i, j * 128:(j + 1) * 128], rhs=vap,
                                    start=(i == 0), stop=(i == 1)),
                                    reads=[f"Pt{i}", vtk], writes=[f"B{PO_}"])
                        o4 = PB[PO_][:, 0:260].rearrange("p (j d) -> p j d", j=4)
                        op("dve", lambda e, g=g, o4=o4: e.tensor_tensor(den, o4[:, :, 64], esink[:, g * 4:(g + 1) * 4], ALU.add),
                           reads=[f"B{PO_}", "esink"], writes=["den"])
                        op("dve", lambda e: e.reciprocal(den, den), reads=["den"], writes=["den"])
                        op("dve", lambda e, g=g, o4=o4: e.tensor_tensor(
                            os_t[:, g * 256:(g + 1) * 256].rearrange("p (j d) -> p j d", j=4), o4[:, :, 0:64],
                            den.unsqueeze(2).to_broadcast([128, 4, 64]), ALU.mult),
                            reads=[f"B{PO_}", "den"], writes=["os_t"])
                    for n in range(4):
                        op("pe", lambda e, n=n: e.transpose(pbf(PT_)[:, n * 128:(n + 1) * 128], os_t[:, n * 128:(n + 1) * 128], ident),
                           reads=["os_t", "ident"], writes=[f"B{PT_}"])
                    op("act", lambda e, qb=qb: e.activation(OsT[:, :, qb * 128:(qb + 1) * 128],
                                                            pbf(PT_)[:, 0:512].rearrange("p (c n) -> p c n", c=4), AF.Copy),
                       reads=[f"B{PT_}"], writes=["OsT"])
                dma("pool", lambda e, k=k: e.dma_start(out=OsTs[k], in_=OsT.rearrange("p c n -> p (c n)")),
                    reads=["OsT"], writes=[f"+OsTs{k}"])

        if dbg:
            dma("pool", lambda e: e.dma_start(out=dbgo["d_cn"], in_=Cn.rearrange("p j h -> p (j h)")), reads=["Cn"])
            dbgo["d_kt"] = dout("d_kt", [65, SV], BF16)
            dbgo["d_qt"] = dout("d_qt", [65, TOWN], BF16)
            dbgo["d_ost"] = dout("d_ost", [128, 2048], BF16)
            dbgo["d_vr"] = dout("d_vr", [128, NT * 8 * 65], BF16)
            dma("sp", lambda e: e.dma_start(out=dbgo["d_kt"], in_=KTs[3]), reads=["+KTs3", "+KTs_ones"])
            dma("sp", lambda e: e.dma_start(out=dbgo["d_qt"], in_=QTs[3]), reads=["+QTs3", "+QTs_c"])
            dma("sp", lambda e: e.dma_start(out=dbgo["d_ost"], in_=OsTs[0]), reads=["+OsTs0"])
            dma("sp", lambda e: e.dma_start(out=dbgo["d_vr"], in_=Vr.rearrange("p j h d -> p (j h d)")), reads=["+Vr", "Vr_valid"])


        A.release()
        S.barrier()

        if stop_after >= 2:
            KT = [A.alloc([SV], BF16) for _ in range(2)]
            QT = [A.alloc([TOWN], BF16) for _ in range(2)]
            ncol = [A.alloc([NT], F32) for _ in range(2)]
            Pq = [A.alloc([512], BF16) for _ in range(3)]
            rdn = A.alloc([512], F32)
            bcs = A.alloc([512], F32)
            ofs = [A.alloc([512], BF16) for _ in range(2)]
            onesr = A.alloc([64], F32)
            op("pool", lambda e: e.memset(onesr, 1.0), writes=["onesr"])
            SB_ = [0, 1, 2]
            OB_ = [3, 4]
            BC_ = 5
            LOOK = 2

            def load_head(h):
                s_ = h % 2
                dma("sp", lambda e: e.dma_start(out=KT[s_][0:65, :], in_=KTs[h]),
                    reads=[f"+KTs{h}", "+KTs_ones"], writes=[f"KT{s_}"])
                dma("sp", lambda e: e.dma_start(out=QT[s_][0:65, :], in_=QTs[h]),
                    reads=[f"+QTs{h}", "+QTs_c"], writes=[f"QT{s_}"])

            units = []
            hk = 0
            for h in range(8):
                for k in range(NOWN):
                    v = 2 * k + 1
                    nkb = 4 * v + 4
                    for kb in range(nkb):
                        units.append(dict(h=h, k=k, v=v, kb=kb, nkb=nkb, hk=hk, first=(k == 0 and kb == 0), last=(kb == nkb - 1)))
                    hk += 1

            def emit_front(i, u):
                h, k, v, kb = u["h"], u["k"], u["v"], u["kb"]
                s_ = h % 2
                if u["first"]:
                    if h == 0:
                        load_head(0)
                    if h + 1 < 8:
                        load_head(h + 1)
                    op("dve", lambda e: e.tensor_scalar(ncol[s_], Cn[:, :, h], -1.0, None, ALU.mult),
                       reads=["Cn"], writes=[f"ncol{s_}"])
                diag = kb >= 4 * v
                q0 = (kb - 4 * v) * 128 if diag else 0
                sb_ = SB_[i % 3]
                pq, pqk = Pq[i % 3], f"Pq{i % 3}"
                op("pe", lambda e: e.matmul(PB[sb_][:, q0:512], lhsT=KT[s_][0:65, kb * 128:(kb + 1) * 128],
                                            rhs=QT[s_][0:65, k * 512 + q0:(k + 1) * 512], start=True, stop=(not diag)),
                   reads=[f"KT{s_}", f"QT{s_}"], writes=[f"B{sb_}"])
                if diag:
                    op("pe", lambda e: e.matmul(PB[sb_][:, q0:q0 + 128], lhsT=ident, rhs=mcur4[:, 0, :],
                                                start=False, stop=True),
                       reads=["ident", "mcur4"], writes=[f"B{sb_}"])
                op("act", lambda e: e.activation(pq[:, q0:512], PB[sb_][:, q0:512], AF.Exp, bias=ncol[s_][:, kb:kb + 1]),
                   reads=[f"B{sb_}", f"ncol{s_}"], writes=[pqk])

            def emit_back(i, u):
                h, k, v, kb, nkb = u["h"], u["k"], u["v"], u["kb"], u["nkb"]
                diag = kb >= 4 * v
                q0 = (kb - 4 * v) * 128 if diag else 0
                pq, pqk = Pq[i % 3], f"Pq{i % 3}"
                ob = OB_[u["hk"] % 2]
                op("pe", lambda e: e.matmul(PB[ob][0:65, q0:512], lhsT=Vr[:, kb, h, :], rhs=pq[:, q0:512],
                                            start=(kb == 0), stop=(kb == nkb - 1)),
                   reads=[pqk, "+Vr", "Vr_valid"], writes=[f"B{ob}"])
                if u["last"]:
                    op("dve", lambda e: e.reciprocal(rdn[64:65, :], PB[ob][64:65, :]), reads=[f"B{ob}"], writes=["rdn"])
                    op("pe", lambda e: e.matmul(PB[BC_][0:64, :], lhsT=onesr[64:65, 0:64], rhs=rdn[64:65, :], start=True, stop=True),
                       reads=["onesr", "rdn"], writes=[f"B{BC_}"])
                    op("act", lambda e: e.activation(bcs[0:64, :], PB[BC_][0:64, :], AF.Copy), reads=[f"B{BC_}"], writes=["bcs"])
                    of_, ofk = ofs[u["hk"] % 2], f"ofs{u['hk'] % 2}"
                    op("dve", lambda e: e.tensor_tensor(of_[0:64, :], PB[ob][0:64, :], bcs[0:64, :], ALU.mult),
                       reads=[f"B{ob}", "bcs"], writes=[ofk])
                    dma("sp", lambda e: e.dma_start(out=OfTs[h, :, k * 512:(k + 1) * 512], in_=of_[0:64, :]),
                        reads=[ofk], writes=[f"+OfTs{k}"])

            nu = len(units)
            for i in range(nu + LOOK):
                if i < nu:
                    emit_front(i, units[i])
                j = i - LOOK
                if j >= 0:
                    emit_back(j, units[j])
        A.release()
        S.barrier()

        if stop_after >= 3:
            dst = A.alloc([NTO, 2], I32)
            gates = A.alloc([NTO, 2], F32)
            A.mark()
            Wg = A.alloc([8, 2048], BF16)
            Wpf = A.alloc([4, D], BF16)
            Wps = A.alloc([4, D], BF16)
            Wo = A.alloc([8, D], BF16)
            Wr = A.alloc([8, 72], F32)
            brbc = A.alloc([72], F32)
            g2bc = A.alloc([D], F32)
            st3 = [A.alloc([2048], F32) for _ in range(2)]
            si = 0
            for c in range(8):
                s3, s3k = st3[si % 2], f"st3{si % 2}"
                si += 1
                dma("sp", lambda e: e.dma_start(out=s3, in_=w_in[c * 128:(c + 1) * 128, 2312:4360]), writes=[s3k])
                if c % 2 == 0:
                    op("dve", lambda e: e.tensor_scalar(Wg[:, c, :], s3, gA[:, c:c + 1], None, ALU.mult),
                       reads=[s3k, "gA"], writes=["+Wg"])
                else:
                    op("act", lambda e: e.activation(Wg[:, c, :], s3, AF.Copy, scale=gA[:, c:c + 1]),
                       reads=[s3k, "gA"], writes=["+Wg"])
            for (wsrc, wdst, nch, wk) in ((w_pf, Wpf, 4, "+Wpf"), (w_ps, Wps, 4, "+Wps"), (w_out, Wo, 8, "+Wo")):
                for c in range(nch):
                    s3, s3k = st3[si % 2], f"st3{si % 2}"
                    si += 1
                    dma("sp", lambda e: e.dma_start(out=s3[:, 0:D], in_=wsrc[c * 128:(c + 1) * 128, :]), writes=[s3k])
                    if si % 2 == 0:
                        op("dve", lambda e: e.tensor_copy(wdst[:, c, :], s3[:, 0:D]), reads=[s3k], writes=[wk])
                    else:
                        op("act", lambda e: e.activation(wdst[:, c, :], s3[:, 0:D], AF.Copy), reads=[s3k], writes=[wk])
            dma("sp", lambda e: e.dma_start(out=Wr, in_=wr.rearrange("(c p) n -> p c n", p=128)), writes=["Wr"])
            dma("sp", lambda e: e.dma_start(out=brbc, in_=br.partition_broadcast(128)), writes=["brbc"])
            dma("sp", lambda e: e.dma_start(out=g2bc, in_=ffn_norm.partition_broadcast(128)), writes=["g2bc"])

            hT3 = A.alloc([8, 512], BF16)
            ofT = A.alloc([4, 512], BF16)
            osT = A.alloc([4, 512], BF16)
            mT = A.alloc([8, 512], BF16)
            sgf = A.alloc([512], F32)
            sgs = A.alloc([512], F32)
            ta = A.alloc([512], F32)
            tb = A.alloc([512], F32)
            x3 = [A.alloc([D], F32) for _ in range(2)]
            x2 = [A.alloc([D], F32) for _ in range(2)]
            h2 = [A.alloc([D], F32) for _ in range(2)]
            h2p = [A.alloc([D], F32) for _ in range(2)]
            h2T = A.alloc([8, 128], F32)
            ss2 = A.alloc([1], F32)
            lg = A.alloc([72], F32)
            sm = A.alloc([16], F32)
            eg = A.alloc([8], F32)
            maskg = A.alloc([8], F32)
            pen = A.alloc([8], F32)
            le = A.alloc([64], F32)
            le2 = A.alloc([64], F32)
            mask1 = A.alloc([64], F32)
            mask2 = A.alloc([64], F32)
            Mt = A.alloc([64], BF16)
            base = A.alloc([64], F32)
            slotb = A.alloc([64], F32)
            rk = A.alloc([64], F32)
            tv = A.alloc([64], F32)
            t1 = A.alloc([64], F32)
            op("pool", lambda e: e.memset(base, 0.0), writes=["base"])
            op("pool", lambda e: e.iota(slotb, pattern=[[CAP, 64]], base=0, channel_multiplier=0,
                                        allow_small_or_imprecise_dtypes=True), writes=["slotb"])
            PA_, PBm_, PG0_, PG1_, PT0_, PT1_, PO3_, PR_ = 0, 1, 2, 3, 4, 5, 6, 7
            ti = 0
            for k in range(NOWN):
                v = 2 * k + 1
                dma("sp", lambda e: e.dma_start(out=hT3.rearrange("p c n -> p (c n)"), in_=HTs[k]),
                    reads=[f"+HTs{k}"], writes=["hT3"])
                for two in range(2):
                    dma("sp", lambda e: e.dma_start(
                        out=ofT[two * 64:(two + 1) * 64, :, :],
                        in_=OfTs[two::2, :, k * 512:(k + 1) * 512].rearrange("n d t -> d n t")),
                        reads=[f"+OfTs{k}"], writes=["ofT"])
                dma("sp", lambda e: e.dma_start(out=osT.rearrange("p c n -> p (c n)"), in_=OsTs[k]),
                    reads=[f"+OsTs{k}"], writes=["osT"])
                for m in range(8):
                    ms = slice(m * 128, (m + 1) * 128)
                    for n in range(4):
                        op("pe", lambda e: e.matmul(PB[PA_], lhsT=Wpf[:, n, ms], rhs=ofT[:, n, :], start=(n == 0), stop=(n == 3)),
                           reads=["+Wpf", "ofT"], writes=[f"B{PA_}"])
                    for n in range(4):
                        op("pe", lambda e: e.matmul(PB[PBm_], lhsT=Wps[:, n, ms], rhs=osT[:, n, :], start=(n == 0), stop=(n == 3)),
                           reads=["+Wps", "osT"], writes=[f"B{PBm_}"])
                    for c in range(8):
                        op("pe", lambda e: e.matmul(PB[PG0_], lhsT=Wg[:, c, ms], rhs=hT3[:, c, :], start=(c == 0), stop=(c == 7)),
                           reads=["+Wg", "hT3"], writes=[f"B{PG0_}"])
                    for c in range(8):
                        op("pe", lambda e: e.matmul(PB[PG1_], lhsT=Wg[:, c, 1024 + m * 128:1024 + (m + 1) * 128], rhs=hT3[:, c, :],
                                                    start=(c == 0), stop=(c == 7)),
                           reads=["+Wg", "hT3"], writes=[f"B{PG1_}"])
                    op("act", lambda e: e.activation(sgf, PB[PG0_], AF.Sigmoid, bias=bgt[:, m:m + 1]),
                       reads=[f"B{PG0_}", "bgt"], writes=["sgf"])
                    op("act", lambda e: e.activation(sgs, PB[PG1_], AF.Sigmoid, bias=bgt[:, 8 + m:9 + m]),
                       reads=[f"B{PG1_}", "bgt"], writes=["sgs"])
                    op("dve", lambda e: e.tensor_tensor(ta, PB[PA_], sgf, ALU.mult), reads=[f"B{PA_}", "sgf"], writes=["ta"])
                    op("dve", lambda e: e.tensor_tensor(tb, PB[PBm_], sgs, ALU.mult), reads=[f"B{PBm_}", "sgs"], writes=["tb"])
                    op("dve", lambda e: e.tensor_tensor(mT[:, m, :], ta, tb, ALU.add), reads=["ta", "tb"], writes=["mT"])
                for qi in range(4):
                    xs, xsk = x3[ti % 2], f"x3{ti % 2}"
                    x2s, x2k = x2[ti % 2], f"x2{ti % 2}"
                    h2s, h2k = h2[ti % 2], f"h2{ti % 2}"
                    r0 = v * 512 + qi * 128
                    o0 = k * 512 + qi * 128
                    dma("sp", lambda e: e.dma_start(out=xs, in_=xv[r0:r0 + 128, :]), writes=[xsk])
                    for half in range(2):
                        hs = slice(half * 512, (half + 1) * 512)
                        for m in range(8):
                            op("pe", lambda e: e.matmul(PB[PO3_], lhsT=mT[:, m, qi * 128:(qi + 1) * 128], rhs=Wo[:, m, hs],
                                                        start=(m == 0), stop=(m == 7)),
                               reads=["mT", "+Wo"], writes=[f"B{PO3_}"])
                        op("dve", lambda e: e.tensor_tensor(x2s[:, hs], PB[PO3_], xs[:, hs], ALU.add),
                           reads=[f"B{PO3_}", xsk], writes=[x2k])
                    dma("sp", lambda e: e.dma_start(out=X2s[o0:o0 + 128, :], in_=x2s), reads=[x2k], writes=[f"X2s{ti}"])
                    op("act", lambda e: e.activation(h2s, x2s, AF.Square, accum_out=ss2), reads=[x2k], writes=[h2k, "ss2"])
                    op("dve", lambda e: e.tensor_scalar(sm[:, 0:1], ss2, 1.0 / D, EPS, ALU.mult, ALU.add), reads=["ss2"], writes=["sm0"])
                    op("act", lambda e: e.activation(sm[:, 0:1], sm[:, 0:1], AF.Ln), reads=["sm0"], writes=["sm0"])
                    op("act", lambda e: e.activation(sm[:, 0:1], sm[:, 0:1], AF.Exp, scale=-0.5), reads=["sm0"], writes=["sm0"])
                    op("dve", lambda e: e.scalar_tensor_tensor(h2s, x2s, sm[:, 0:1], g2bc, ALU.mult, ALU.mult),
                       reads=[x2k, "sm0", "g2bc", h2k], writes=[h2k])
                    hps, hpk = h2p[ti % 2], f"h2p{ti % 2}"
                    op("pool", lambda e: e.tensor_copy(hps.rearrange("t (c p) -> t c p", c=8),
                                                       h2s.rearrange("t (p c) -> t c p", c=8)),
                       reads=[h2k], writes=[hpk])
                    for c in range(8):
                        bnk = PT0_ if c < 4 else PT1_
                        op("pe", lambda e: e.transpose(PB[bnk][:, (c % 4) * 128:(c % 4 + 1) * 128], h2s[:, c * 128:(c + 1) * 128], identf),
                           reads=[h2k, "identf"], writes=[f"B{bnk}"])
                    op("act", lambda e: e.activation(h2T[:, 0:4, :], PB[PT0_].rearrange("p (c n) -> p c n", c=4), AF.Copy),
                       reads=[f"B{PT0_}"], writes=["h2Ta"])
                    op("dve", lambda e: e.tensor_copy(h2T[:, 4:8, :], PB[PT1_].rearrange("p (c n) -> p c n", c=4)),
                       reads=[f"B{PT1_}"], writes=["h2Tb"])
                    for c in range(8):
                        op("pe", lambda e: e.matmul(PB[PR_][:, 0:72], lhsT=h2T[:, c, :], rhs=Wr[:, c, :], start=(c == 0), stop=(c == 7)),
                           reads=["h2Ta", "h2Tb", "Wr"], writes=[f"B{PR_}"])
                    op("dve", lambda e: e.tensor_tensor(lg, PB[PR_][:, 0:72], brbc, ALU.add), reads=[f"B{PR_}", "brbc"], writes=["lg"])
                    op("dve", lambda e: e.reduce_max(sm[:, 1:2], lg[:, 0:8], AX.X), reads=["lg"], writes=["sm1"])
                    op("dve", lambda e: e.tensor_scalar(sm[:, 2:3], sm[:, 1:2], -1.0, None, ALU.mult), reads=["sm1"], writes=["sm2"])
                    op("act", lambda e: e.activation(eg, lg[:, 0:8], AF.Exp, bias=sm[:, 2:3], accum_out=sm[:, 3:4]),
                       reads=["lg", "sm2"], writes=["eg", "sm3"])
                    op("dve", lambda e: e.reciprocal(sm[:, 4:5], sm[:, 3:4]), reads=["sm3"], writes=["sm4"])
                    op("dve", lambda e: e.tensor_scalar(maskg, lg[:, 0:8], sm[:, 1:2], None, ALU.is_equal), reads=["lg", "sm1"], writes=["maskg"])
                    op("dve", lambda e: e.tensor_scalar(pen, maskg, 1e9, -1e9, ALU.mult, ALU.add), reads=["maskg"], writes=["pen"])
                    op("dve", lambda e: e.tensor_tensor(le.rearrange("p (g j) -> p g j", g=8), lg[:, 8:72].rearrange("p (g j) -> p g j", g=8),
                                                        pen.unsqueeze(2).to_broadcast([128, 8, 8]), ALU.add),
                       reads=["lg", "pen"], writes=["le"])
                    op("dve", lambda e: e.reduce_max(sm[:, 5:6], le, AX.X), reads=["le"], writes=["sm5"])
                    op("dve", lambda e: e.tensor_scalar(mask1, le, sm[:, 5:6], None, ALU.is_equal), reads=["le", "sm5"], writes=["mask1"])
                    op("dve", lambda e: e.scalar_tensor_tensor(le2, mask1, -1e9, le, ALU.mult, ALU.add), reads=["mask1", "le"], writes=["le2"])
                    op("dve", lambda e: e.reduce_max(sm[:, 6:7], le2, AX.X), reads=["le2"], writes=["sm6"])
                    op("dve", lambda e: e.tensor_scalar(mask2, le2, sm[:, 6:7], None, ALU.is_equal), reads=["le2", "sm6"], writes=["mask2"])
                    op("dve", lambda e: e.tensor_tensor(sm[:, 7:8], sm[:, 6:7], sm[:, 5:6], ALU.subtract), reads=["sm5", "sm6"], writes=["sm7"])
                    op("act", lambda e: e.activation(sm[:, 8:9], sm[:, 7:8], AF.Exp), reads=["sm7"], writes=["sm8"])
                    op("dve", lambda e: e.tensor_scalar(sm[:, 8:9], sm[:, 8:9], 1.0, None, ALU.add), reads=["sm8"], writes=["sm8"])
                    op("dve", lambda e: e.reciprocal(sm[:, 9:10], sm[:, 8:9]), reads=["sm8"], writes=["sm9"])
                    op("dve", lambda e: e.tensor_tensor(gates[:, ti, 0:1], sm[:, 9:10], sm[:, 4:5], ALU.mult), reads=["sm9", "sm4"], writes=["gates"])
                    op("dve", lambda e: e.tensor_tensor(gates[:, ti, 1:2], sm[:, 4:5], gates[:, ti, 0:1], ALU.subtract), reads=["gates", "sm4"], writes=["gates"])
                    op("dve", lambda e: e.tensor_tensor(Mt, mask1, mask2, ALU.add), reads=["mask1", "mask2"], writes=["Mt"])
                    op("pe", lambda e: e.matmul(PB[PR_][:, 128:192], lhsT=ustrict, rhs=Mt, start=True, stop=True),
                       reads=["ustrict", "Mt"], writes=[f"B{PR_}"])
                    op("pe", lambda e: e.matmul(PB[PR_][:, 256:320], lhsT=ones_bf, rhs=Mt, start=True, stop=True),
                       reads=["ones_bf", "Mt"], writes=[f"B{PR_}"])
                    op("dve", lambda e: e.tensor_tensor(rk, PB[PR_][:, 128:192], base, ALU.add), reads=[f"B{PR_}", "base"], writes=["rk"])
                    op("dve", lambda e: e.tensor_tensor(base, PB[PR_][:, 256:320], base, ALU.add), reads=[f"B{PR_}", "base"], writes=["base"])
                    op("dve", lambda e: e.tensor_scalar(tv, rk, float(CAP), 1e6, ALU.is_ge, ALU.mult), reads=["rk"], writes=["tv"])
                    op("dve", lambda e: e.tensor_tensor(tv, tv, rk, ALU.add), reads=["tv", "rk"], writes=["tv"])
                    op("dve", lambda e: e.tensor_tensor(tv, tv, slotb, ALU.add), reads=["tv", "slotb"], writes=["tv"])
                    for (mk, mkk, col) in ((mask1, "mask1", 0), (mask2, "mask2", 1)):
                        op("dve", lambda e: e.tensor_tensor(t1, mk, tv, ALU.mult), reads=[mkk, "tv"], writes=["t1"])
                        op("dve", lambda e: e.reduce_sum(sm[:, 10 + col:11 + col], t1, AX.X), reads=["t1"], writes=[f"sm1{col}"])
                        op("dve", lambda e: e.tensor_copy(dst[:, ti, col:col + 1], sm[:, 10 + col:11 + col]), reads=[f"sm1{col}"], writes=["dst"])
                        dma("pool", lambda e: e.indirect_dma_start(
                            out=Xd, out_offset=bass.IndirectOffsetOnAxis(ap=dst[:, ti, col:col + 1], axis=0),
                            in_=hps, in_offset=None, bounds_check=NSLOT - 1, oob_is_err=False),
                            reads=[hpk, "dst"], writes=["+Xd"])
                    ti += 1
            A.release()
            S.barrier()
            if dbg:
                dma("sp", lambda e: e.dma_start(out=dbgo["d_dst"], in_=dst.rearrange("p t c -> p (t c)")), reads=["dst"])
                dma("sp", lambda e: e.dma_start(out=dbgo["d_gate"], in_=gates.rearrange("p t c -> p (t c)")), reads=["gates"])
                dma("sp", lambda e: e.dma_start(out=dbgo["d_x2"], in_=X2s), reads=[f"X2s{i_}" for i_ in range(NTO)])

        if stop_after >= 4:
            A.mark()
            Sg1 = [A.alloc([8, 512], F32) for _ in range(2)]
            Sg3 = [A.alloc([8, 512], F32) for _ in range(2)]
            Sg2 = [A.alloc([4, D], F32) for _ in range(2)]
            R1 = [A.alloc([8, 512], BF16) for _ in range(2)]
            R3 = [A.alloc([8, 512], BF16) for _ in range(2)]
            R2 = [A.alloc([4, D], BF16) for _ in range(2)]
            xe = [A.alloc([D], F32) for _ in range(4)]
            xeT = A.alloc([8, CAP], BF16)
            hidT = A.alloc([4, CAP], BF16)
            sil2 = [A.alloc([512], F32) for _ in range(2)]
            hidtm = [A.alloc([512], BF16) for _ in range(2)]
            ye = [A.alloc([D], F32) for _ in range(2)]
            NR = CAP // 128

            def load_expert(ex):
                s_ = ex % 2
                dma("sp", lambda e: e.dma_start(out=Sg1[s_], in_=w1[ex].rearrange("(p c) n -> p c n", p=128)), writes=[f"Sg1{s_}"])
                dma("sp", lambda e: e.dma_start(out=Sg3[s_], in_=w3[ex].rearrange("(p c) n -> p c n", p=128)), writes=[f"Sg3{s_}"])
                dma("sp", lambda e: e.dma_start(out=Sg2[s_], in_=w2[ex].rearrange("(c p) n -> p c n", p=128)), writes=[f"Sg2{s_}"])

            def round_expert(ex):
                s_ = ex % 2
                op("dve", lambda e: e.tensor_copy(R1[s_], Sg1[s_]), reads=[f"Sg1{s_}"], writes=[f"R1{s_}"])
                op("pool", lambda e: e.tensor_copy(R3[s_], Sg3[s_]), reads=[f"Sg3{s_}"], writes=[f"R3{s_}"])
                op("act", lambda e: e.activation(R2[s_], Sg2[s_], AF.Copy), reads=[f"Sg2{s_}"], writes=[f"R2{s_}"])

            load_expert(0)
            if NEXP > 1:
                load_expert(1)
            round_expert(0)
            xi = 0
            yi = 0
            def load_x_rows(ex):
                for r in range(NR):
                    q_ = (ex * NR + r) % 4
                    dma("sp", lambda e: e.dma_start(out=xe[q_], in_=Xd[ex * CAP + r * 128:ex * CAP + (r + 1) * 128, :]),
                        reads=["+Xd"], writes=[f"xe{q_}"])

            load_x_rows(0)
            for ex in range(NEXP):
                s_ = ex % 2
                if ex + 1 < NEXP:
                    load_x_rows(ex + 1)
                if ex + 2 < NEXP:
                    load_expert(ex + 2)
                for r in range(NR):
                    q_ = (ex * NR + r) % 4
                    xs, xsk = xe[q_], f"xe{q_}"
                    for c in range(8):
                        bnk = 0 if c < 4 else 1
                        op("pe", lambda e: e.transpose(PB[bnk][:, (c % 4) * 128:(c % 4 + 1) * 128], xs[:, c * 128:(c + 1) * 128], identf),
                           reads=[xsk, "identf"], writes=[f"B{bnk}"])
                    op("act", lambda e: e.activation(xeT[:, 0:4, r * 128:(r + 1) * 128], PB[0].rearrange("p (c n) -> p c n", c=4), AF.Copy),
                       reads=["B0"], writes=["xeTa"])
                    op("dve", lambda e: e.tensor_copy(xeT[:, 4:8, r * 128:(r + 1) * 128], PB[1].rearrange("p (c n) -> p c n", c=4)),
                       reads=["B1"], writes=["xeTb"])
                for r in range(NR):
                    b1, b3 = (2, 3) if r % 2 == 0 else (4, 5)
                    for (Wt, wk, bb) in ((R1[s_], f"R1{s_}", b1), (R3[s_], f"R3{s_}", b3)):
                        for c in range(8):
                            op("pe", lambda e: e.matmul(PB[bb], lhsT=xeT[:, c, r * 128:(r + 1) * 128], rhs=Wt[:, c, :],
                                                        start=(c == 0), stop=(c == 7)),
                               reads=[wk, "xeTa", "xeTb"], writes=[f"B{bb}"])
                    sl_, slk = sil2[r % 2], f"sil{r % 2}"
                    hm, hmk = hidtm[r % 2], f"hidtm{r % 2}"
                    op("act", lambda e: e.activation(sl_, PB[b1], AF.Silu), reads=[f"B{b1}"], writes=[slk])
                    op("dve", lambda e: e.tensor_tensor(hm, sl_, PB[b3], ALU.mult), reads=[slk, f"B{b3}"], writes=[hmk])
                    for m in range(4):
                        op("pe", lambda e: e.transpose(pbf(0)[:, r * 512 + m * 128:r * 512 + (m + 1) * 128],
                                                       hm[:, m * 128:(m + 1) * 128], ident),
                           reads=[hmk, "ident"], writes=["B0"])
                    if r % 2 == 0:
                        op("act", lambda e: e.activation(hidT[:, :, r * 128:(r + 1) * 128],
                                                         pbf(0)[:, r * 512:(r + 1) * 512].rearrange("p (m n) -> p m n", m=4), AF.Copy),
                           reads=["B0"], writes=[f"hidT{r}"])
                    else:
                        op("dve", lambda e: e.tensor_copy(hidT[:, :, r * 128:(r + 1) * 128],
                                                          pbf(0)[:, r * 512:(r + 1) * 512].rearrange("p (m n) -> p m n", m=4)),
                           reads=["B0"], writes=[f"hidT{r}"])
                if ex + 1 < NEXP:
                    round_expert(ex + 1)
                for r in range(NR):
                    ys, ysk = ye[yi % 2], f"+ye{yi % 2}"
                    yi += 1
                    for half in range(2):
                        bb = 6 + half
                        for m in range(4):
                            op("pe", lambda e: e.matmul(PB[bb], lhsT=hidT[:, m, r * 128:(r + 1) * 128],
                                                        rhs=R2[s_][:, m, half * 512:(half + 1) * 512],
                                                        start=(m == 0), stop=(m == 3)),
                               reads=[f"hidT{r}", f"R2{s_}"], writes=[f"B{bb}"])
                        if half == 0:
                            op("act", lambda e: e.activation(ys[:, 0:512], PB[bb], AF.Copy), reads=[f"B{bb}"], writes=[ysk])
                        else:
                            op("dve", lambda e: e.tensor_copy(ys[:, 512:1024], PB[bb]), reads=[f"B{bb}"], writes=[ysk])
                    dma("sp", lambda e: e.dma_start(out=Yd[ex * CAP + r * 128:ex * CAP + (r + 1) * 128, :], in_=ys),
                        reads=[ysk], writes=["+Yd"])
            A.release()
            S.barrier()

        if stop_after >= 5:
            A.mark()
            gfbc = A.alloc([D], F32)
            dma("sp", lambda e: e.dma_start(out=gfbc, in_=final_norm.partition_broadcast(128)), writes=["gfbc"])
            y1 = [A.alloc([D], F32) for _ in range(2)]
            y2 = [A.alloc([D], F32) for _ in range(2)]
            xc = [A.alloc([D], F32) for _ in range(2)]
            oc = [A.alloc([D], F32) for _ in range(2)]
            jk = A.alloc([D], F32)
            s5 = A.alloc([2], F32)
            for ti in range(NTO):
                u = ti % 2
                op("pool", lambda e: e.memset(y1[u], 0.0), writes=[f"y1{u}"])
                op("pool", lambda e: e.memset(y2[u], 0.0), writes=[f"y2{u}"])
                for (yt_, ytk, col) in ((y1[u], f"y1{u}", 0), (y2[u], f"y2{u}", 1)):
                    dma("pool", lambda e: e.indirect_dma_start(
                        out=yt_, out_offset=None, in_=Yd,
                        in_offset=bass.IndirectOffsetOnAxis(ap=dst[:, ti, col:col + 1], axis=0),
                        bounds_check=NSLOT - 1, oob_is_err=False),
                        reads=["+Yd", "dst"], writes=[ytk])
                dma("sp", lambda e: e.dma_start(out=xc[u], in_=X2s[ti * 128:(ti + 1) * 128, :]), reads=[f"X2s{ti}"], writes=[f"xc{u}"])
                op("dve", lambda e: e.scalar_tensor_tensor(oc[u], y1[u], gates[:, ti, 0:1], xc[u], ALU.mult, ALU.add),
                   reads=[f"y1{u}", "gates", f"xc{u}"], writes=[f"oc{u}"])
                op("dve", lambda e: e.scalar_tensor_tensor(oc[u], y2[u], gates[:, ti, 1:2], oc[u], ALU.mult, ALU.add),
                   reads=[f"y2{u}", "gates", f"oc{u}"], writes=[f"oc{u}"])
                op("act", lambda e: e.activation(jk, oc[u], AF.Square, accum_out=s5[:, 0:1]), reads=[f"oc{u}"], writes=["jk", "s50"])
                op("dve", lambda e: e.tensor_scalar(s5[:, 1:2], s5[:, 0:1], 1.0 / D, EPS, ALU.mult, ALU.add), reads=["s50"], writes=["s51"])
                op("act", lambda e: e.activation(s5[:, 1:2], s5[:, 1:2], AF.Ln), reads=["s51"], writes=["s51"])
                op("act", lambda e: e.activation(s5[:, 1:2], s5[:, 1:2], AF.Exp, scale=-0.5), reads=["s51"], writes=["s51"])
                op("dve", lambda e: e.scalar_tensor_tensor(oc[u], oc[u], s5[:, 1:2], gfbc, ALU.mult, ALU.mult),
                   reads=[f"oc{u}", "s51", "gfbc"], writes=[f"oc{u}"])
                dma("sp", lambda e: e.dma_start(out=y[ti * 128:(ti + 1) * 128, :], in_=oc[u]), reads=[f"oc{u}"], writes=["+y"])
            A.release()

        S.finish("sp")
        S.emit()
        print("arena high water (KiB):", A.hi / 256, "instr counts:", {k_: len(v_) for k_, v_ in S.ops.items()})
    return nc


def _prep_inputs(inp):
    x = np.asarray(inp["x"], np.float32)
    pos = np.asarray(inp["positions"], np.int32)
    L = 0
    w_in = np.ascontiguousarray(np.asarray(inp["w_in"], np.float32)[L])
    shared = {
        "w_in": w_in,
        "attn_norm": np.ascontiguousarray(np.asarray(inp["attn_norm"], np.float32)[L].reshape(8, 128).T),
        "b_forget": np.ascontiguousarray(np.asarray(inp["b_forget"], np.float32)[L].reshape(8, 1)),
        "b_gate": np.ascontiguousarray(np.asarray(inp["b_gate"], np.float32)[L].reshape(16, 128).T),
        "sinks": np.ascontiguousarray(np.asarray(inp["attn_sinks"], np.float32)[L].reshape(1, 8)),
        "w_pf": np.ascontiguousarray(np.asarray(inp["w_proj_fox"], np.float32)[L]),
        "w_ps": np.ascontiguousarray(np.asarray(inp["w_proj_swa"], np.float32)[L]),
        "w_out": np.ascontiguousarray(np.asarray(inp["w_out"], np.float32)[L]),
        "ffn_norm": np.ascontiguousarray(np.asarray(inp["ffn_norm"], np.float32)[L].reshape(1, D)),
        "wr": np.ascontiguousarray(np.concatenate([np.asarray(inp["w_group"], np.float32)[L],
                                                   np.asarray(inp["w_expert"], np.float32)[L]], axis=1)),
        "br": np.ascontiguousarray(np.concatenate([np.asarray(inp["b_group"], np.float32)[L],
                                                   np.asarray(inp["b_expert"], np.float32)[L]]).reshape(1, 72)),
        "w1": np.ascontiguousarray(np.asarray(inp["w1"], np.float32)[L]),
        "w3": np.ascontiguousarray(np.asarray(inp["w3"], np.float32)[L]),
        "w2": np.ascontiguousarray(np.asarray(inp["w2"], np.float32)[L]),
        "final_norm": np.ascontiguousarray(np.asarray(inp["final_norm"], np.float32).reshape(1, D)),
    }
    half = 32
    invf = (10000.0 ** (-(np.arange(half, dtype=np.float32)) * 2.0 / 64)).astype(np.float32)
    shared["invf"] = np.ascontiguousarray(invf[np.arange(128) % 32].reshape(128, 1))
    maps = []
    for core in range(8):
        b, p = core // 2, core % 2
        m = dict(shared)
        if p == 1:
            m["xv"] = np.ascontiguousarray(x[b, :SV])
            m["posv"] = np.ascontiguousarray(pos[b, :SV].reshape(1, SV))
            vflag = np.ones(SV, np.float32)
        else:
            xz = np.zeros((SV, D), np.float32)
            xz[512:] = x[b, :SV - 512]
            m["xv"] = xz
            pz = np.zeros((1, SV), np.int32)
            pz[0, 512:] = pos[b, :SV - 512]
            m["posv"] = pz
            vflag = np.ones(SV, np.float32)
            vflag[:512] = 0.0
        m["valid"] = np.ascontiguousarray(vflag.reshape(NT, 128).T)
        maps.append(m)
    return maps


_NC_CACHE = {}


def kernel(**inputs):
    maps = _prep_inputs(inputs)
    if "nc" not in _NC_CACHE:
        _NC_CACHE["nc"] = build()
    nc = _NC_CACHE["nc"]
    res = run_bass_kernel_spmd(nc, maps, core_ids=list(range(8)))
    B, Sq = 4, 8192
    out = np.zeros((B, Sq, D), np.float32)
    for core in range(8):
        b, p = core // 2, core % 2
        yc = np.asarray(res.results[core]["y"]).reshape(NOWN, 512, D)
        for k in range(NOWN):
            v = 2 * k + 1
            r0 = v * 512 if p == 1 else (v - 1) * 512
            out[b, r0:r0 + 512] = yc[k]
    return out
```
